# Optimizing a Trainium2 kernel written in Bass

```python
import math
import jax, jax.numpy as jnp
from jax import lax
import numpy as np

D_MODEL = 1024
BATCH = 2
SEQ = 8192
DEPTH = 2

N_EVEN = (DEPTH + 1) // 2
N_ODD = DEPTH // 2
N_SUB = 3
D_FF = 2816
EPS = 1e-6

POOL_WINDOWS = (2, 4, 8, 16)
POOL_GROUPS = len(POOL_WINDOWS)
POOL_WIDTH = D_MODEL // 2
POOL_GROUP_DIM = POOL_WIDTH // POOL_GROUPS

DIFF_HEADS = 4
DIFF_HEAD_DIM = 64
DIFF_V_DIM = 2 * DIFF_HEAD_DIM
DIFF_QK_WIDTH = DIFF_HEADS * 2 * DIFF_HEAD_DIM
DIFF_WIDTH = DIFF_HEADS * DIFF_V_DIM
Q_BLOCK = 128

EVEN_IN = POOL_WIDTH + 2 * DIFF_QK_WIDTH + DIFF_WIDTH
EVEN_MIX = POOL_WIDTH + DIFF_WIDTH

MLSTM_HEADS = 4
MLSTM_QK_DIM = 128
MLSTM_V_DIM = 256
MLSTM_CHUNK = 128
MLSTM_QK_WIDTH = MLSTM_HEADS * MLSTM_QK_DIM
MLSTM_V_WIDTH = MLSTM_HEADS * MLSTM_V_DIM
ODD_IN = 2 * MLSTM_QK_WIDTH + 2 * MLSTM_V_WIDTH + 2 * MLSTM_HEADS
ODD_MIX = MLSTM_V_WIDTH

kernel_name = "hybrid_pool_diffattn_mlstm_macaron_adaln"


def rmsnorm(x, g):
    xf = x.astype(jnp.float32)
    y = xf * lax.rsqrt(jnp.mean(xf * xf, axis=-1, keepdims=True) + EPS)
    return (y * g.astype(jnp.float32)).astype(x.dtype)


def modnorm(x, g, shift, scale):
    return rmsnorm(x, g) * (1 + scale[:, None, :]) + shift[:, None, :]


def swiglu(h, w_gate, w_up, w_down):
    return (jax.nn.silu(h @ w_gate) * (h @ w_up)) @ w_down


def pool_mixer(u, w_group, scale):
    B, S, _ = u.shape
    ug = u.astype(jnp.float32).reshape(B, S, POOL_GROUPS, POOL_GROUP_DIM)
    cs = jnp.cumsum(ug, axis=1)
    pos = jnp.arange(S)
    means = []
    for g, w in enumerate(POOL_WINDOWS):
        csg = cs[:, :, g]
        cs_pad = jnp.pad(csg, ((0, 0), (w, 0), (0, 0)))
        total = cs_pad[:, w:] - cs_pad[:, :S]
        count = jnp.minimum(pos + 1, w).astype(jnp.float32)
        means.append(total / count[None, :, None])
    pooled = jnp.stack(means, axis=2) - ug
    y = jnp.einsum('bsgc,gcd->bsgd', pooled, w_group.astype(jnp.float32))
    y = y.reshape(B, S, POOL_WIDTH) * scale.astype(jnp.float32)
    return y.astype(u.dtype)


def diff_attention(q, k, v, lam_params, subln_g, lambda_init):
    B, S = q.shape[0], q.shape[1]
    nb = S // Q_BLOCK
    lp = lam_params.astype(jnp.float32)
    lam = jnp.exp(jnp.sum(lp[0] * lp[1])) - jnp.exp(jnp.sum(lp[2] * lp[3])) + lambda_init
    kf = k.astype(jnp.float32)
    vf = v.astype(jnp.float32)
    qb = (q.astype(jnp.float32) * DIFF_HEAD_DIM ** -0.5).reshape(
        B, nb, Q_BLOCK, DIFF_HEADS, 2, DIFF_HEAD_DIM).transpose(1, 0, 2, 3, 4, 5)
    key_pos = jnp.arange(S)

    def block(args):
        i, qblk = args
        qpos = i * Q_BLOCK + jnp.arange(Q_BLOCK)
        s = jnp.einsum('bqhcd,bkhcd->bhcqk', qblk, kf)
        s = jnp.where(key_pos[None, :] <= qpos[:, None], s, -jnp.inf)
        p = jax.nn.softmax(s, axis=-1)
        a = p[:, :, 0] - lam * p[:, :, 1]
        return jnp.einsum('bhqk,bkhe->bqhe', a, vf)

    o = lax.map(block, (jnp.arange(nb), qb))
    o = o.transpose(1, 0, 2, 3, 4).reshape(B, S, DIFF_HEADS, DIFF_V_DIM)
    o = rmsnorm(o, subln_g) * (1.0 - lambda_init)
    return o.reshape(B, S, DIFF_WIDTH).astype(q.dtype)


def mlstm_chunkwise(q, k, v, i_pre, f_pre):
    B, H, S, dk = q.shape
    dv = v.shape[-1]
    L = MLSTM_CHUNK
    nc = S // L
    q = q.reshape(B, H, nc, L, dk)
    k = (k * dk ** -0.5).reshape(B, H, nc, L, dk)
    v = v.reshape(B, H, nc, L, dv)
    ig = i_pre.reshape(B, H, nc, L)
    logf = jax.nn.log_sigmoid(f_pre).reshape(B, H, nc, L)
    b = jnp.cumsum(logf, axis=-1)
    b_last = b[..., -1]

    a = b_last[..., None] - b + ig
    m_loc = jnp.max(a, axis=-1)
    w = jnp.exp(a - m_loc[..., None])
    C_loc = jnp.einsum('bhclv,bhclk->bhcvk', v * w[..., None], k)
    n_loc = jnp.einsum('bhcl,bhclk->bhck', w, k)

    def step(carry, inp):
        C, n, m = carry
        bl, ml, Cl, nl = inp
        m_new = jnp.maximum(bl + m, ml)
        s_old = jnp.exp(bl + m - m_new)
        s_loc = jnp.exp(ml - m_new)
        C_new = s_old[..., None, None] * C + s_loc[..., None, None] * Cl
        n_new = s_old[..., None] * n + s_loc[..., None] * nl
        return (C_new, n_new, m_new), (C, n, m)

    init = (jnp.zeros((B, H, dv, dk), jnp.float32),
            jnp.zeros((B, H, dk), jnp.float32),
            jnp.zeros((B, H), jnp.float32))
    xs = (jnp.moveaxis(b_last, 2, 0), jnp.moveaxis(m_loc, 2, 0),
          jnp.moveaxis(C_loc, 2, 0), jnp.moveaxis(n_loc, 2, 0))
    _, (C_prev, n_prev, m_prev) = lax.scan(step, init, xs)
    C_prev = jnp.moveaxis(C_prev, 0, 2)
    n_prev = jnp.moveaxis(n_prev, 0, 2)
    m_prev = jnp.moveaxis(m_prev, 0, 2)

    causal = jnp.tril(jnp.ones((L, L), dtype=bool))
    D = b[..., :, None] - b[..., None, :] + ig[..., None, :]
    D = jnp.where(causal, D, -jnp.inf)
    inter_log = b + m_prev[..., None]
    m_t = jnp.maximum(inter_log, jnp.max(D, axis=-1))
    Dw = jnp.exp(D - m_t[..., None])
    inter_w = jnp.exp(inter_log - m_t)
    s = jnp.einsum('bhctd,bhcsd->bhcts', q, k) * Dw
    num = jnp.einsum('bhcts,bhcsv->bhctv', s, v) + \
        inter_w[..., None] * jnp.einsum('bhctk,bhcvk->bhctv', q, C_prev)
    den = jnp.sum(s, axis=-1) + inter_w * jnp.einsum('bhctk,bhck->bhct', q, n_prev)
    h = num / jnp.maximum(jnp.abs(den), jnp.exp(-m_t))[..., None]
    return h.reshape(B, H, S, dv)


def even_mixer(h, w_in, w_pool, pool_scale, lam_params, subln_g, w_out, lambda_init):
    B, S, _ = h.shape
    p = h @ w_in
    u = p[..., :POOL_WIDTH]
    o1 = POOL_WIDTH
    q = p[..., o1:o1 + DIFF_QK_WIDTH].reshape(B, S, DIFF_HEADS, 2, DIFF_HEAD_DIM)
    o2 = o1 + DIFF_QK_WIDTH
    k = p[..., o2:o2 + DIFF_QK_WIDTH].reshape(B, S, DIFF_HEADS, 2, DIFF_HEAD_DIM)
    o3 = o2 + DIFF_QK_WIDTH
    v = p[..., o3:o3 + DIFF_WIDTH].reshape(B, S, DIFF_HEADS, DIFF_V_DIM)
    y_pool = pool_mixer(u, w_pool, pool_scale)
    y_diff = diff_attention(q, k, v, lam_params, subln_g, lambda_init)
    y = jnp.concatenate([y_pool, y_diff.astype(y_pool.dtype)], axis=-1)
    return y @ w_out


def odd_mixer(h, w_in, b_gates, norm_g, w_out):
    B, S, _ = h.shape
    p = h @ w_in
    nq = MLSTM_QK_WIDTH
    nv = MLSTM_V_WIDTH
    q = p[..., :nq]
    k = p[..., nq:2 * nq]
    v = p[..., 2 * nq:2 * nq + nv]
    o = p[..., 2 * nq + nv:2 * nq + 2 * nv]
    gates = p[..., 2 * nq + 2 * nv:].astype(jnp.float32).reshape(B, S, 2, MLSTM_HEADS) + \
        b_gates.astype(jnp.float32)
    i_pre = gates[:, :, 0].transpose(0, 2, 1)
    f_pre = gates[:, :, 1].transpose(0, 2, 1)

    def heads(t, d):
        return t.astype(jnp.float32).reshape(B, S, MLSTM_HEADS, d).transpose(0, 2, 1, 3)

    ht = mlstm_chunkwise(heads(q, MLSTM_QK_DIM), heads(k, MLSTM_QK_DIM),
                         heads(v, MLSTM_V_DIM), i_pre, f_pre)
    ht = ht.transpose(0, 2, 1, 3)
    ht = rmsnorm(ht, norm_g.reshape(MLSTM_HEADS, MLSTM_V_DIM))
    y = jax.nn.sigmoid(o.astype(jnp.float32)) * ht.reshape(B, S, nv)
    return y.astype(h.dtype) @ w_out


def setup_inputs(seed: int = 0) -> dict:
    key = jax.random.key(seed)
    ks = jax.random.split(key, 24)
    f32 = jnp.float32

    def nrm(k, shape, fan_in, gain=1.0):
        return jax.random.normal(k, shape, f32) * (gain * fan_in ** -0.5)

    def near_one(k, shape):
        return 1.0 + 0.02 * jax.random.normal(k, shape, f32)

    x = jax.random.normal(ks[0], (BATCH, SEQ, D_MODEL), f32)
    c = jax.random.normal(ks[1], (BATCH, D_MODEL), f32)
    w_mod = nrm(ks[2], (DEPTH, D_MODEL, N_SUB * 3 * D_MODEL), D_MODEL, 0.5)
    b_mod = 0.02 * jax.random.normal(ks[3], (DEPTH, N_SUB * 3 * D_MODEL), f32)
    norm_g = near_one(ks[4], (DEPTH, N_SUB, D_MODEL))
    w_ffn_gate = nrm(ks[5], (DEPTH, 2, D_MODEL, D_FF), D_MODEL)
    w_ffn_up = nrm(ks[6], (DEPTH, 2, D_MODEL, D_FF), D_MODEL)
    w_ffn_down = nrm(ks[7], (DEPTH, 2, D_FF, D_MODEL), D_FF)
    w_in_even = nrm(ks[8], (N_EVEN, D_MODEL, EVEN_IN), D_MODEL)
    w_pool = nrm(ks[9], (N_EVEN, POOL_GROUPS, POOL_GROUP_DIM, POOL_GROUP_DIM), POOL_GROUP_DIM)
    pool_scale = near_one(ks[10], (N_EVEN, POOL_WIDTH))
    diff_lambda = 0.1 * jax.random.normal(ks[11], (N_EVEN, 4, DIFF_HEAD_DIM), f32)
    diff_subln_g = near_one(ks[12], (N_EVEN, DIFF_V_DIM))
    w_out_even = nrm(ks[13], (N_EVEN, EVEN_MIX, D_MODEL), EVEN_MIX)
    w_in_odd = nrm(ks[14], (N_ODD, D_MODEL, ODD_IN), D_MODEL)
    b_i = 0.1 * jax.random.normal(ks[15], (N_ODD, MLSTM_HEADS), f32)
    b_f = jnp.linspace(3.0, 6.0, MLSTM_HEADS, dtype=f32)[None, :] + \
        0.1 * jax.random.normal(ks[16], (N_ODD, MLSTM_HEADS), f32)
    b_gates_odd = jnp.stack([b_i, b_f], axis=1)
    mlstm_norm_g = near_one(ks[17], (N_ODD, ODD_MIX))
    w_out_odd = nrm(ks[18], (N_ODD, ODD_MIX, D_MODEL), ODD_MIX)
    final_g = near_one(ks[19], (D_MODEL,))
    return {"x": x, "c": c, "w_mod": w_mod, "b_mod": b_mod, "norm_g": norm_g,
            "w_ffn_gate": w_ffn_gate, "w_ffn_up": w_ffn_up, "w_ffn_down": w_ffn_down,
            "w_in_even": w_in_even, "w_pool": w_pool, "pool_scale": pool_scale,
            "diff_lambda": diff_lambda, "diff_subln_g": diff_subln_g, "w_out_even": w_out_even,
            "w_in_odd": w_in_odd, "b_gates_odd": b_gates_odd, "mlstm_norm_g": mlstm_norm_g,
            "w_out_odd": w_out_odd, "final_g": final_g}


def reference(x, c, w_mod, b_mod, norm_g, w_ffn_gate, w_ffn_up, w_ffn_down,
              w_in_even, w_pool, pool_scale, diff_lambda, diff_subln_g, w_out_even,
              w_in_odd, b_gates_odd, mlstm_norm_g, w_out_odd, final_g):
    B = x.shape[0]
    c_act = jax.nn.silu(c)
    for l in range(DEPTH):
        mod = (c_act @ w_mod[l] + b_mod[l]).reshape(B, N_SUB, 3, D_MODEL)
        shift, scale, gate = mod[:, :, 0], mod[:, :, 1], mod[:, :, 2]
        h = modnorm(x, norm_g[l, 0], shift[:, 0], scale[:, 0])
        x = x + 0.5 * gate[:, 0, None, :] * swiglu(h, w_ffn_gate[l, 0], w_ffn_up[l, 0], w_ffn_down[l, 0])
        h = modnorm(x, norm_g[l, 1], shift[:, 1], scale[:, 1])
        if l % 2 == 0:
            e = l // 2
            lambda_init = 0.8 - 0.6 * math.exp(-0.3 * l)
            y = even_mixer(h, w_in_even[e], w_pool[e], pool_scale[e], diff_lambda[e],
                           diff_subln_g[e], w_out_even[e], lambda_init)
        else:
            o = l // 2
            y = odd_mixer(h, w_in_odd[o], b_gates_odd[o], mlstm_norm_g[o], w_out_odd[o])
        x = x + gate[:, 1, None, :] * y
        h = modnorm(x, norm_g[l, 2], shift[:, 2], scale[:, 2])
        x = x + 0.5 * gate[:, 2, None, :] * swiglu(h, w_ffn_gate[l, 1], w_ffn_up[l, 1], w_ffn_down[l, 1])
    return rmsnorm(x, final_g)
```

```python
import numpy as np
import ml_dtypes
from contextlib import ExitStack
import concourse.bass as bass
import concourse.mybir as mybir
from concourse.bass_utils import run_bass_kernel_spmd

F32 = mybir.dt.float32
BF16 = mybir.dt.bfloat16
AF = mybir.ActivationFunctionType
ALU = mybir.AluOpType
NPBF = ml_dtypes.bfloat16

NCORES = 8
D = 1024
KC = 8
SEQ = 8192
NT = 2048
TT = 512
NTILE = NT // TT
DFF = 2816
FC = DFF // 128
EPS = 1e-6
GF = 2


class Buf:
    __slots__ = ("w", "r", "name")

    def __init__(self, name=""):
        self.w = None
        self.r = []
        self.name = name


class FW:
    def __init__(self, nc, es, ndma=8):
        self.nc = nc
        self.engs = {"pe": nc.tensor, "dve": nc.vector, "act": nc.scalar, "pool": nc.gpsimd, "sp": nc.sync}
        self.sem = {k: es.enter_context(nc.semaphore("s_" + k)) for k in self.engs}
        self.cnt = {k: 0 for k in self.engs}
        self.pending = {k: False for k in self.engs}
        self.seen = {k: {} for k in self.engs}
        self.dsem = {}
        self.dn = {}
        self.ndma = ndma
        for q in ("sp", "pool", "act"):
            self.dsem[q] = [es.enter_context(nc.semaphore("d_%s%d" % (q, i))) for i in range(ndma)]
            self.dn[q] = 0

    def _wait(self, e, toks):
        need = {}
        for t in toks:
            if t is None:
                continue
            s, v = t
            if e == "pe" and s is self.sem["pe"]:
                continue
            if need.get(id(s), (None, 0))[1] < v:
                need[id(s)] = (s, v)
        eng = self.engs[e]
        for k, (s, v) in need.items():
            if self.seen[e].get(k, 0) >= v:
                continue
            eng.wait_ge(s, v)
            self.seen[e][k] = v

    @staticmethod
    def _deps(reads, writes):
        toks = []
        for b in reads:
            toks.append(b.w)
        for b in writes:
            toks.append(b.w)
            toks.extend(b.r)
        return toks

    @staticmethod
    def _update(tok, reads, writes):
        for b in reads:
            b.r.append(tok)
            if len(b.r) > 64:
                best = {}
                for s, v in b.r:
                    if best.get(id(s), (None, 0))[1] < v:
                        best[id(s)] = (s, v)
                b.r = list(best.values())
        for b in writes:
            b.w = tok
            b.r = []

    def op(self, e, fn, reads=(), writes=(), signal=True):
        signal = True
        self._wait(e, self._deps(reads, writes))
        ins = fn()
        if signal:
            self.cnt[e] += 1
            ins.then_inc(self.sem[e], 1)
            self.pending[e] = False
            tok = (self.sem[e], self.cnt[e])
        else:
            self.pending[e] = True
            tok = (self.sem[e], self.cnt[e] + 1)
        self._update(tok, reads, writes)
        return tok

    def dma(self, q, out, in_, reads=(), writes=(), **kw):
        n = self.dn[q]
        s = self.dsem[q][n % self.ndma]
        prev = 16 * (n // self.ndma)
        toks = self._deps(reads, writes)
        if prev > 0:
            toks.append((s, prev))
        self._wait(q, toks)
        self.engs[q].dma_start(out=out, in_=in_, **kw).then_inc(s, 16)
        self.dn[q] = n + 1
        tok = (s, prev + 16)
        self._update(tok, reads, writes)
        return tok

    def all_tokens(self):
        toks = []
        for k in self.engs:
            assert not self.pending[k], k
            if self.cnt[k] > 0:
                toks.append((self.sem[k], self.cnt[k]))
        for q in self.dsem:
            n = self.dn[q]
            for i in range(self.ndma):
                uses = (n - i + self.ndma - 1) // self.ndma if n > i else 0
                if uses > 0:
                    toks.append((self.dsem[q][i], 16 * uses))
        return toks

    def barrier(self, engines=("pe", "dve", "act", "pool", "sp")):
        toks = self.all_tokens()
        for e in engines:
            self._wait(e, toks)

    def finish(self):
        self._wait("sp", self.all_tokens())


class K:
    def __init__(self, name):
        self.nc = bass.Bass("TRN2", target_bir_lowering=False)
        self.es = ExitStack()
        self.fw = FW(self.nc, self.es)
        self.name = name
        self.ins = {}
        self.outs = {}
        nc = self.nc
        self.ps = [self.es.enter_context(nc.psum_tensor("ps%d" % i, [128, 512], F32)) for i in range(8)]
        self.psb = [Buf("ps%d" % i) for i in range(8)]
        self._rot = {}
        self.uid = 0

    def inp(self, name, shape, dt=F32):
        t = self.nc.dram_tensor(name, list(shape), dt, kind="ExternalInput").ap()
        self.ins[name] = (tuple(shape), dt)
        return t

    def out(self, name, shape, dt=F32):
        t = self.nc.dram_tensor(name, list(shape), dt, kind="ExternalOutput").ap()
        self.outs[name] = (tuple(shape), dt)
        return t

    def sb(self, es, name, shape, dt):
        self.uid += 1
        return es.enter_context(self.nc.sbuf_tensor("%s_%d" % (name, self.uid), list(shape), dt))

    def rot(self, key, n):
        i = self._rot.get(key, 0)
        self._rot[key] = i + 1
        return i % n

    def bank(self, role):
        base = {"g": 0, "u": 2, "d": 4, "s": 6}[role]
        i = base + self.rot("bank_" + role, 2)
        return self.ps[i], self.psb[i]


def load_consts(k, es):
    nc, fw = k.nc, k.fw
    c = {}
    c["ones_d"] = k.sb(es, "ones_d", [128, 128], BF16)
    c["ones_d_b"] = Buf()
    fw.op("dve", lambda: nc.vector.memset(c["ones_d"][:], 1.0 / 1024.0), writes=[c["ones_d_b"]])
    c["eps"] = k.sb(es, "eps", [128, 1], F32)
    fw.op("dve", lambda: nc.vector.memset(c["eps"][:], EPS), writes=[c["ones_d_b"]])
    return c


def emit_mod(k, es, c_in, wmod_l, bmod_l, ng_l, cst):
    nc, fw = k.nc, k.fw
    cs = k.sb(es, "c_sb", [128, 8], F32)
    cb = k.sb(es, "c_bf", [128, 8], BF16)
    sg = k.sb(es, "c_sg", [128, 8], F32)
    b_c = Buf()
    b_cb = Buf()
    fw.dma("sp", cs[:], c_in, writes=[b_c])
    fw.op("act", lambda: nc.scalar.activation(out=sg[:], in_=cs[:], func=AF.Sigmoid), reads=[b_c], writes=[b_cb])
    fw.op("dve", lambda: nc.vector.tensor_tensor(out=cb[:], in0=cs[:], in1=sg[:], op=ALU.mult), reads=[b_c, b_cb], writes=[b_cb])
    modsb = k.sb(es, "modsb", [128, 72], F32)
    bmsb = k.sb(es, "bmsb", [128, 72], F32)
    ngsb = k.sb(es, "ngsb", [128, 24], F32)
    b_mod = Buf()
    b_bm = Buf()
    fw.dma("sp", bmsb[:], bmod_l, writes=[b_bm])
    fw.dma("sp", ngsb[:], ng_l, writes=[b_bm])
    wv = wmod_l.rearrange("(kc p) n -> p kc n", p=128)
    with ExitStack() as es2:
        wblk = [k.sb(es2, "wmodblk", [128, 8, 1024], BF16) for _ in range(2)]
        wb_b = [Buf(), Buf()]
        ps, psb = k.ps[7], k.psb[7]
        for blk in range(9):
            i = blk % 2
            fw.dma("pool", wblk[i][:], wv[:, :, blk * 1024:(blk + 1) * 1024], writes=[wb_b[i]])
            for jc in range(8):
                col = blk * 8 + jc
                for kc in range(8):
                    last = (kc == 7 and jc == 7)
                    fw.op("pe", lambda i=i, jc=jc, kc=kc, col=col: nc.tensor.matmul(
                        ps[:, col:col + 1], lhsT=wblk[i][:, kc, jc * 128:(jc + 1) * 128], rhs=cb[:, kc:kc + 1],
                        start=(kc == 0), stop=(kc == 7)),
                        reads=[wb_b[i], b_cb], writes=[psb], signal=last)
        fw.op("dve", lambda: nc.vector.tensor_tensor(out=modsb[:], in0=ps[:, 0:72], in1=bmsb[:], op=ALU.add),
              reads=[psb, b_bm], writes=[b_mod])
        fw.barrier()
    m = {"buf": b_mod}
    gs = k.sb(es, "gs", [128, 24], F32)
    gt = k.sb(es, "gt", [128, 24], F32)
    for s in range(3):
        sc = modsb[:, (s * 3 + 1) * 8:(s * 3 + 2) * 8]
        g = ngsb[:, s * 8:(s + 1) * 8]
        fw.op("dve", lambda s=s, sc=sc, g=g: nc.vector.scalar_tensor_tensor(
            out=gs[:, s * 8:(s + 1) * 8], in0=sc, scalar=1.0, in1=g, op0=ALU.add, op1=ALU.mult),
            reads=[b_mod, b_bm], writes=[b_mod])
        ga = modsb[:, (s * 3 + 2) * 8:(s * 3 + 3) * 8]
        fw.op("dve", lambda s=s, ga=ga: nc.vector.tensor_scalar(
            out=gt[:, s * 8:(s + 1) * 8], in0=ga, scalar1=(1.0 if s == 1 else 0.5), scalar2=None, op0=ALU.mult),
            reads=[b_mod], writes=[b_mod])
    m["gs"] = [gs[:, s * 8:(s + 1) * 8] for s in range(3)]
    m["sh"] = [modsb[:, (s * 3) * 8:(s * 3 + 1) * 8] for s in range(3)]
    m["gt"] = [gt[:, s * 8:(s + 1) * 8] for s in range(3)]
    return m


class XS:
    def __init__(self, k, es):
        self.t = k.sb(es, "xT", [128, KC, NT], F32)
        self.b = [[Buf("x%d_%d" % (t, c)) for c in range(KC)] for t in range(NTILE)]

    def load(self, k, src):
        v = src.rearrange("(c p) t -> p c t", p=128)
        for t in range(NTILE):
            k.fw.dma("sp", self.t[:, :, t * TT:(t + 1) * TT], v[:, :, t * TT:(t + 1) * TT], writes=self.b[t])

    def store(self, k, dst):
        v = dst.rearrange("(c p) t -> p c t", p=128)
        for t in range(NTILE):
            k.fw.dma("sp", v[:, :, t * TT:(t + 1) * TT], self.t[:, :, t * TT:(t + 1) * TT], reads=self.b[t])


class NormScratch:
    def __init__(self, k, es):
        self.sq = [k.sb(es, "sq", [128, TT], BF16) for _ in range(3)]
        self.sqb = [Buf() for _ in range(3)]
        self.tmp = [k.sb(es, "ntmp", [128, TT], F32) for _ in range(2)]
        self.tmpb = [Buf() for _ in range(2)]
        self.rs = [k.sb(es, "rstd", [128, TT], F32) for _ in range(2)]
        self.rsb = [Buf() for _ in range(2)]


def emit_norm_tile(k, cst, ns, xs, t, hT, hcol, hb, gs, sh, mb):
    nc, fw = k.nc, k.fw
    ps, psb = k.bank("s")
    tok = slice(t * TT, (t + 1) * TT)
    for c in range(KC):
        i = k.rot("sq", 3)
        fw.op("act", lambda c=c, i=i: nc.scalar.activation(out=ns.sq[i][:], in_=xs.t[:, c, tok], func=AF.Square),
              reads=[xs.b[t][c]], writes=[ns.sqb[i]])
        fw.op("pe", lambda c=c, i=i: nc.tensor.matmul(ps[:], lhsT=cst["ones_d"][:], rhs=ns.sq[i][:],
                                                       start=(c == 0), stop=(c == KC - 1)),
              reads=[ns.sqb[i], cst["ones_d_b"]], writes=[psb], signal=(c == KC - 1))
    r = k.rot("rs", 2)
    fw.op("act", lambda: nc.scalar.activation(out=ns.rs[r][:], in_=ps[:], func=AF.Sqrt, bias=cst["eps"][:, 0:1]),
          reads=[psb, cst["ones_d_b"]], writes=[ns.rsb[r]])
    fw.op("dve", lambda: nc.vector.reciprocal(out=ns.rs[r][:], in_=ns.rs[r][:]),
          reads=[ns.rsb[r]], writes=[ns.rsb[r]])
    for c in range(KC):
        i = k.rot("ntmp", 2)
        fw.op("dve", lambda c=c, i=i: nc.vector.tensor_tensor(out=ns.tmp[i][:], in0=xs.t[:, c, tok], in1=ns.rs[r][:],
                                                              op=ALU.mult),
              reads=[xs.b[t][c], ns.rsb[r]], writes=[ns.tmpb[i]])
        fw.op("act", lambda c=c, i=i: nc.scalar.activation(out=hT[:, c, hcol:hcol + TT], in_=ns.tmp[i][:],
                                                           func=AF.Identity, scale=gs[:, c:c + 1], bias=sh[:, c:c + 1]),
              reads=[ns.tmpb[i], mb], writes=[hb])


def emit_ffn(k, es_outer, cst, ns, xs, mod, s, wg, wu, wd):
    nc, fw = k.nc, k.fw
    with ExitStack() as es:
        hT = k.sb(es, "hT", [128, KC, 2 * TT], BF16)
        hb = [Buf(), Buf()]
        act = k.sb(es, "act", [128, FC, 2 * TT], BF16)
        actb = [[Buf() for _ in range(2)] for _ in range(FC)]
        wdt = k.sb(es, "wd", [128, FC, D], BF16)
        wdb = [Buf() for _ in range(FC)]
        NG = FC // GF
        wgt = [k.sb(es, "wg", [128, KC, GF * 128], BF16) for _ in range(2)]
        wut = [k.sb(es, "wu", [128, KC, GF * 128], BF16) for _ in range(2)]
        wgb = [Buf(), Buf()]
        sgt = [k.sb(es, "sgt", [128, TT], F32) for _ in range(2)]
        sgb = [Buf(), Buf()]
        wgv = wg.rearrange("(kc p) n -> p kc n", p=128)
        wuv = wu.rearrange("(kc p) n -> p kc n", p=128)
        wdv = wd.rearrange("(f p) n -> p f n", p=128)
        for f in range(0, FC, 2):
            fw.dma("pool", wdt[:, f:f + 2, :], wdv[:, f:f + 2, :], writes=wdb[f:f + 2])
        for st in range(2):
            for tl in range(2):
                emit_norm_tile(k, cst, ns, xs, st * 2 + tl, hT, tl * TT, hb[tl], mod["gs"][s], mod["sh"][s], mod["buf"])
            for g in range(NG):
                i = k.rot("wgu", 2)
                cols = slice(g * GF * 128, (g + 1) * GF * 128)
                fw.dma("pool", wgt[i][:], wgv[:, :, cols], writes=[wgb[i]])
                fw.dma("pool", wut[i][:], wuv[:, :, cols], writes=[wgb[i]])
                for fl in range(GF):
                    f = g * GF + fl
                    for tl in range(2):
                        pg, pgb = k.bank("g")
                        pu, pub = k.bank("u")
                        for kc in range(KC):
                            fw.op("pe", lambda kc=kc, i=i, fl=fl, tl=tl, pg=pg: nc.tensor.matmul(
                                pg[:], lhsT=wgt[i][:, kc, fl * 128:(fl + 1) * 128], rhs=hT[:, kc, tl * TT:(tl + 1) * TT],
                                start=(kc == 0), stop=(kc == KC - 1)),
                                reads=[wgb[i], hb[tl]], writes=[pgb], signal=(kc == KC - 1))
                        for kc in range(KC):
                            fw.op("pe", lambda kc=kc, i=i, fl=fl, tl=tl, pu=pu: nc.tensor.matmul(
                                pu[:], lhsT=wut[i][:, kc, fl * 128:(fl + 1) * 128], rhs=hT[:, kc, tl * TT:(tl + 1) * TT],
                                start=(kc == 0), stop=(kc == KC - 1)),
                                reads=[wgb[i], hb[tl]], writes=[pub], signal=(kc == KC - 1))
                        j = k.rot("sgt", 2)
                        fw.op("act", lambda j=j, pg=pg: nc.scalar.activation(out=sgt[j][:], in_=pg[:], func=AF.Silu),
                              reads=[pgb], writes=[sgb[j]])
                        fw.op("dve", lambda j=j, pu=pu, f=f, tl=tl: nc.vector.tensor_tensor(
                            out=act[:, f, tl * TT:(tl + 1) * TT], in0=pu[:], in1=sgt[j][:], op=ALU.mult),
                            reads=[pub, sgb[j]], writes=[actb[f][tl]])
            for dc in range(KC):
                for tl in range(2):
                    t = st * 2 + tl
                    pd, pdb = k.bank("d")
                    for f in range(FC):
                        fw.op("pe", lambda f=f, dc=dc, tl=tl, pd=pd: nc.tensor.matmul(
                            pd[:], lhsT=wdt[:, f, dc * 128:(dc + 1) * 128], rhs=act[:, f, tl * TT:(tl + 1) * TT],
                            start=(f == 0), stop=(f == FC - 1)),
                            reads=[wdb[f], actb[f][tl]], writes=[pdb], signal=(f == FC - 1))
                    xsl = xs.t[:, dc, t * TT:(t + 1) * TT]
                    fw.op("dve", lambda pd=pd, dc=dc, xsl=xsl: nc.vector.scalar_tensor_tensor(
                        out=xsl, in0=pd[:], scalar=mod["gt"][s][:, dc:dc + 1], in1=xsl, op0=ALU.mult, op1=ALU.add),
                        reads=[pdb, mod["buf"], xs.b[t][dc]], writes=[xs.b[t][dc]])
        fw.barrier()


def emit_inproj_even(k, cst, ns, xs, mod, w_in, pf, pt):
    nc, fw = k.nc, k.fw
    with ExitStack() as es:
        hT = k.sb(es, "hTi", [128, KC, 2 * TT], BF16)
        hb = [Buf(), Buf()]
        wt = k.sb(es, "win", [128, KC, 2048], BF16)
        wb = [Buf() for _ in range(4)]
        wv = w_in.rearrange("(kc p) n -> p kc n", p=128)
        for g in range(4):
            fw.dma("pool", wt[:, :, g * 512:(g + 1) * 512], wv[:, :, g * 512:(g + 1) * 512], writes=[wb[g]])
        stg_t = [k.sb(es, "stg_t", [128, 1024], BF16) for _ in range(2)]
        stg_tb = [Buf(), Buf()]
        stg_f = [k.sb(es, "stg_f", [128, 2 * TT], BF16) for _ in range(2)]
        stg_fb = [Buf(), Buf()]
        outb = Buf()
        for st in range(2):
            for tl in range(2):
                emit_norm_tile(k, cst, ns, xs, st * 2 + tl, hT, tl * TT, hb[tl], mod["gs"][1], mod["sh"][1], mod["buf"])
            for tc in range(8):
                tl = tc // 4
                j = k.rot("stg_t", 2)
                for half, g in enumerate((0, 3)):
                    ps, psb = k.bank("g" if half == 0 else "u")
                    for kc in range(KC):
                        fw.op("pe", lambda kc=kc, tc=tc, g=g, ps=ps: nc.tensor.matmul(
                            ps[:], lhsT=hT[:, kc, tc * 128:(tc + 1) * 128], rhs=wt[:, kc, g * 512:(g + 1) * 512],
                            start=(kc == 0), stop=(kc == KC - 1)),
                            reads=[hb[tl], wb[g]], writes=[psb], signal=(kc == KC - 1))
                    eng = "act" if half == 0 else "dve"
                    if eng == "act":
                        fw.op("act", lambda j=j, half=half, ps=ps: nc.scalar.copy(out=stg_t[j][:, half * 512:(half + 1) * 512], in_=ps[:]),
                              reads=[psb], writes=[stg_tb[j]])
                    else:
                        fw.op("dve", lambda j=j, half=half, ps=ps: nc.vector.tensor_copy(out=stg_t[j][:, half * 512:(half + 1) * 512], in_=ps[:]),
                              reads=[psb], writes=[stg_tb[j]])
                r0 = st * 2 * TT + tc * 128
                fw.dma("sp", pt[r0:r0 + 128, :], stg_t[j][:], reads=[stg_tb[j]], writes=[outb])
            for kind in range(2):
                g = 1 + kind
                for h in range(4):
                    j = k.rot("stg_f", 2)
                    for tl in range(2):
                        ps, psb = k.bank("d")
                        for kc in range(KC):
                            fw.op("pe", lambda kc=kc, g=g, h=h, tl=tl, ps=ps: nc.tensor.matmul(
                                ps[:], lhsT=wt[:, kc, g * 512 + h * 128:g * 512 + (h + 1) * 128],
                                rhs=hT[:, kc, tl * TT:(tl + 1) * TT], start=(kc == 0), stop=(kc == KC - 1)),
                                reads=[hb[tl], wb[g]], writes=[psb], signal=(kc == KC - 1))
                        if tl == 0:
                            fw.op("act", lambda j=j, tl=tl, ps=ps: nc.scalar.copy(out=stg_f[j][:, tl * TT:(tl + 1) * TT], in_=ps[:]),
                                  reads=[psb], writes=[stg_fb[j]])
                        else:
                            fw.op("dve", lambda j=j, tl=tl, ps=ps: nc.vector.tensor_copy(out=stg_f[j][:, tl * TT:(tl + 1) * TT], in_=ps[:]),
                                  reads=[psb], writes=[stg_fb[j]])
                    fw.dma("sp", pf[kind * 4 + h, :, st * 2 * TT:(st + 1) * 2 * TT], stg_f[j][:], reads=[stg_fb[j]], writes=[outb])
        fw.barrier()


def build_L1():
    k = K("L1")
    xin = k.inp("xT", [D, NT])
    c_in = k.inp("c", [128, 8])
    wmod = k.inp("w_mod0", [D, 9 * D])
    bmod = k.inp("b_mod0", [128, 72])
    ng = k.inp("ng0", [128, 24])
    wg = k.inp("wg", [D, DFF])
    wu = k.inp("wu", [D, DFF])
    wd = k.inp("wd", [DFF, D])
    w_in = k.inp("w_in", [D, 2048])
    xout = k.out("xo", [D, NT])
    pf = k.out("pf", [8, 128, NT], BF16)
    pt = k.out("pt", [NT, 1024], BF16)
    with k.es as es:
        cst = load_consts(k, es)
        xs = XS(k, es)
        xs.load(k, xin)
        ns = NormScratch(k, es)
        import os
        stages = os.environ.get("STAGES", "mod,ffn,inproj").split(",")
        mod = emit_mod(k, es, c_in, wmod, bmod, ng, cst)
        if "ffn" in stages:
            emit_ffn(k, es, cst, ns, xs, mod, 0, wg, wu, wd)
        if "inproj" in stages:
            emit_inproj_even(k, cst, ns, xs, mod, w_in, pf, pt)
        xs.store(k, xout)
        k.fw.finish()
    return k


_CACHE = {}


def get_prog(name, builder):
    if name not in _CACHE:
        _CACHE[name] = builder()
    return _CACHE[name]


def run(k, in_maps):
    res = run_bass_kernel_spmd(k.nc, in_maps, core_ids=list(range(NCORES)))
    return res.results


def fm(a):
    return np.ascontiguousarray(a.reshape(-1, 8, 128).transpose(2, 0, 1).reshape(128, -1))


def kernel(**inp):
    return kernel_unfused(**inp)


LAMBDA_INIT0 = 0.8 - 0.6 * 1.0


def emit_mixer_even(k, es0, qT, kT, v_tok, u_tok, tri_in, bands_in, wpool_in, pscale_in, lam_in, subg_in, yT):
    nc, fw = k.nc, k.fw
    NQT = SEQ // TT
    NKC = SEQ // 128
    with ExitStack() as es:
        q1 = k.sb(es, "q1pad", [128, SEQ], BF16)
        q2 = k.sb(es, "q2pad", [128, SEQ], BF16)
        kt = k.sb(es, "kt", [128, SEQ], BF16)
        vt = k.sb(es, "vt", [128, NKC, 128], BF16)
        ut = k.sb(es, "ut", [128, NKC, 128], BF16)
        b_q = [Buf() for _ in range(NQT)]
        b_k = [Buf() for _ in range(4)]
        b_v = [Buf() for _ in range(4)]
        b_u = [Buf() for _ in range(4)]
        b_c = Buf()
        tri = k.sb(es, "tri", [128, 128], BF16)
        bands = k.sb(es, "bands", [128, 3, 128], BF16)
        wp = k.sb(es, "wp", [128, 128], BF16)
        psc = k.sb(es, "psc", [128, 1], F32)
        lam = k.sb(es, "lam", [128, 256], F32)
        subg = k.sb(es, "subg", [128, 1], F32)
        ones = k.sb(es, "ones1", [128, 128], BF16)
        ones_e = k.sb(es, "ones_e", [128, 128], BF16)
        epsb = k.sb(es, "eps2", [128, 1], F32)
        fw.dma("sp", tri[:], tri_in, writes=[b_c])
        fw.dma("sp", bands[:], bands_in, writes=[b_c])
        fw.dma("pool", wp[:], wpool_in, writes=[b_c])
        fw.dma("sp", psc[:], pscale_in, writes=[b_c])
        fw.dma("sp", lam[:], lam_in, writes=[b_c])
        fw.dma("sp", subg[:], subg_in, writes=[b_c])
        fw.op("dve", lambda: nc.vector.memset(ones[:], 1.0), writes=[b_c])
        fw.op("dve", lambda: nc.vector.memset(ones_e[:], 1.0 / 128.0), writes=[b_c])
        fw.op("dve", lambda: nc.vector.memset(epsb[:], EPS), writes=[b_c])
        uv = u_tok.rearrange("(c p) e -> p c e", p=128)
        vv = v_tok.rearrange("(c p) e -> p c e", p=128)
        for i in range(4):
            fw.dma("sp", ut[:, i * 16:(i + 1) * 16, :], uv[:, i * 16:(i + 1) * 16, :], writes=[b_u[i]])
        for i in range(4):
            fw.dma("sp", kt[:, i * 2048:(i + 1) * 2048], kT[:, i * 2048:(i + 1) * 2048], writes=[b_k[i]])
            fw.dma("sp", vt[:, i * 16:(i + 1) * 16, :], vv[:, i * 16:(i + 1) * 16, :], writes=[b_v[i]])
        fw.op("dve", lambda: nc.vector.memset(q1[64:128, :], 0.0), writes=b_q)
        fw.op("dve", lambda: nc.vector.memset(q2[0:64, :], 0.0), writes=b_q)
        for i in range(4):
            sl = slice(i * 2048, (i + 1) * 2048)
            fw.dma("sp", q1[0:64, sl], qT[0:64, sl], writes=b_q[i * 4:(i + 1) * 4])
            fw.dma("sp", q2[64:128, sl], qT[64:128, sl], writes=b_q[i * 4:(i + 1) * 4])
        ltmp = k.sb(es, "ltmp", [128, 128], F32)
        lsum = k.sb(es, "lsum", [128, 2], F32)
        nlam = k.sb(es, "nlam", [128, 1], F32)
        gsub = k.sb(es, "gsub", [128, 1], F32)
        b_l = Buf()
        fw.op("dve", lambda: nc.vector.tensor_tensor(out=ltmp[:, 0:64], in0=lam[:, 0:64], in1=lam[:, 64:128], op=ALU.mult),
              reads=[b_c], writes=[b_l])
        fw.op("dve", lambda: nc.vector.tensor_tensor(out=ltmp[:, 64:128], in0=lam[:, 128:192], in1=lam[:, 192:256], op=ALU.mult),
              reads=[b_c, b_l], writes=[b_l])
        fw.op("dve", lambda: nc.vector.reduce_sum(out=lsum[:, 0:1], in_=ltmp[:, 0:64], axis=mybir.AxisListType.X),
              reads=[b_l], writes=[b_l])
        fw.op("dve", lambda: nc.vector.reduce_sum(out=lsum[:, 1:2], in_=ltmp[:, 64:128], axis=mybir.AxisListType.X),
              reads=[b_l], writes=[b_l])
        fw.op("act", lambda: nc.scalar.activation(out=lsum[:], in_=lsum[:], func=AF.Exp), reads=[b_l], writes=[b_l])
        fw.op("dve", lambda: nc.vector.tensor_tensor(out=nlam[:], in0=lsum[:, 1:2], in1=lsum[:, 0:1], op=ALU.subtract),
              reads=[b_l], writes=[b_l])
        fw.op("dve", lambda: nc.vector.tensor_scalar(out=nlam[:], in0=nlam[:], scalar1=-LAMBDA_INIT0, scalar2=None, op0=ALU.add),
              reads=[b_l], writes=[b_l])
        fw.op("dve", lambda: nc.vector.tensor_scalar(out=gsub[:], in0=subg[:], scalar1=1.0 - LAMBDA_INIT0, scalar2=None, op0=ALU.mult),
              reads=[b_c, b_l], writes=[b_l])
        pst = [k.sb(es, "pool_sb", [128, TT], BF16) for _ in range(2)]
        pstb = [Buf(), Buf()]
        yst = [k.sb(es, "ystg", [128, TT], BF16) for _ in range(2)]
        ystb = [Buf(), Buf()]
        outb = Buf()
        for qi in range(NQT):
            ps, psb = k.bank("g")
            for cc in range(4):
                tcn = qi * 4 + cc
                osl = ps[:, cc * 128:(cc + 1) * 128]
                bsel = 2 if tcn == 0 else 0
                fw.op("pe", lambda tcn=tcn, osl=osl, bsel=bsel: nc.tensor.matmul(
                    osl, lhsT=ut[:, tcn, :], rhs=bands[:, bsel, :], start=True, stop=(tcn == 0)),
                    reads=[b_u[tcn // 16], b_c], writes=[psb])
                if tcn > 0:
                    fw.op("pe", lambda tcn=tcn, osl=osl: nc.tensor.matmul(
                        osl, lhsT=ut[:, tcn - 1, :], rhs=bands[:, 1, :], start=False, stop=True),
                        reads=[b_u[(tcn - 1) // 16], b_c], writes=[psb])
            j = k.rot("pool_sb", 2)
            fw.op("dve", lambda j=j, ps=ps: nc.vector.tensor_copy(out=pst[j][:], in_=ps[:]), reads=[psb], writes=[pstb[j]])
            ps2, ps2b = k.bank("u")
            fw.op("pe", lambda j=j, ps2=ps2: nc.tensor.matmul(ps2[:], lhsT=wp[:], rhs=pst[j][:], start=True, stop=True),
                  reads=[pstb[j], b_c], writes=[ps2b])
            jj = k.rot("ystg", 2)
            fw.op("act", lambda jj=jj, ps2=ps2: nc.scalar.activation(out=yst[jj][:], in_=ps2[:], func=AF.Copy, scale=psc[:, 0:1]),
                  reads=[ps2b, b_c], writes=[ystb[jj]])
            fw.dma("sp", yT[0, :, qi * TT:(qi + 1) * TT], yst[jj][:], reads=[ystb[jj]], writes=[outb])
        NP = 3
        p1 = [k.sb(es, "p1", [128, TT], BF16) for _ in range(NP)]
        p2 = [k.sb(es, "p2", [128, TT], BF16) for _ in range(NP)]
        p1b = [Buf() for _ in range(NP)]
        p2b = [Buf() for _ in range(NP)]
        r1 = k.sb(es, "r1", [128, TT], F32)
        r2 = k.sb(es, "r2", [128, TT], F32)
        o_sb = k.sb(es, "o_sb", [128, TT], F32)
        osq = k.sb(es, "osq", [128, TT], BF16)
        rs = k.sb(es, "rs2", [128, TT], F32)
        b_e = Buf()
        O1, O1b = k.ps[4], k.psb[4]
        O2, O2b = k.ps[5], k.psb[5]
        D1, D1b = k.ps[6], k.psb[6]
        D2, D2b = k.ps[7], k.psb[7]
        for qi in range(NQT):
            nkc = 4 * qi + 4
            q0 = qi * TT
            for kc in range(nkc):
                o = kc - 4 * qi
                c0 = 0 if o < 0 else o * 128
                cols = slice(c0, TT)
                qcols = slice(q0 + c0, q0 + TT)
                sa, sab = k.bank("g")
                sb_, sbb = k.bank("u")
                ksl = kt[:, kc * 128:(kc + 1) * 128]
                fw.op("pe", lambda sa=sa, ksl=ksl, qcols=qcols, cols=cols: nc.tensor.matmul(
                    sa[:, cols], lhsT=ksl, rhs=q1[:, qcols], start=True, stop=True),
                    reads=[b_k[kc // 16], b_q[qi]], writes=[sab])
                fw.op("pe", lambda sb_=sb_, ksl=ksl, qcols=qcols, cols=cols: nc.tensor.matmul(
                    sb_[:, cols], lhsT=ksl, rhs=q2[:, qcols], start=True, stop=True),
                    reads=[b_k[kc // 16], b_q[qi]], writes=[sbb])
                i = k.rot("prot", NP)
                fw.op("act", lambda i=i, sa=sa, cols=cols: nc.scalar.activation(
                    out=p1[i][:, cols], in_=sa[:, cols], func=AF.Exp, scale=0.125), reads=[sab], writes=[p1b[i]])
                fw.op("act", lambda i=i, sb_=sb_, cols=cols: nc.scalar.activation(
                    out=p2[i][:, cols], in_=sb_[:, cols], func=AF.Exp, scale=0.125), reads=[sbb], writes=[p2b[i]])
                if o >= 0:
                    dsl = slice(c0, c0 + 128)
                    fw.op("dve", lambda i=i, dsl=dsl: nc.vector.tensor_tensor(
                        out=p1[i][:, dsl], in0=p1[i][:, dsl], in1=tri[:], op=ALU.mult), reads=[p1b[i], b_c], writes=[p1b[i]])
                    fw.op("dve", lambda i=i, dsl=dsl: nc.vector.tensor_tensor(
                        out=p2[i][:, dsl], in0=p2[i][:, dsl], in1=tri[:], op=ALU.mult), reads=[p2b[i], b_c], writes=[p2b[i]])
                first, last = (kc == 0), (kc == nkc - 1)
                vsl = vt[:, kc, :]
                fw.op("pe", lambda i=i, vsl=vsl, cols=cols: nc.tensor.matmul(
                    O1[:, cols], lhsT=vsl, rhs=p1[i][:, cols], start=first, stop=last),
                    reads=[b_v[kc // 16], p1b[i]], writes=[O1b])
                fw.op("pe", lambda i=i, vsl=vsl, cols=cols: nc.tensor.matmul(
                    O2[:, cols], lhsT=vsl, rhs=p2[i][:, cols], start=first, stop=last),
                    reads=[b_v[kc // 16], p2b[i]], writes=[O2b])
                fw.op("pe", lambda i=i, cols=cols: nc.tensor.matmul(
                    D1[:, cols], lhsT=ones[:], rhs=p1[i][:, cols], start=first, stop=last),
                    reads=[b_c, p1b[i]], writes=[D1b])
                fw.op("pe", lambda i=i, cols=cols: nc.tensor.matmul(
                    D2[:, cols], lhsT=ones[:], rhs=p2[i][:, cols], start=first, stop=last),
                    reads=[b_c, p2b[i]], writes=[D2b])
            fw.op("dve", lambda: nc.vector.reciprocal(out=r1[:], in_=D1[:]), reads=[D1b], writes=[b_e])
            fw.op("dve", lambda: nc.vector.reciprocal(out=r2[:], in_=D2[:]), reads=[D2b, b_e], writes=[b_e])
            fw.op("dve", lambda: nc.vector.tensor_tensor(out=r1[:], in0=O1[:], in1=r1[:], op=ALU.mult), reads=[O1b, b_e], writes=[b_e])
            fw.op("dve", lambda: nc.vector.tensor_tensor(out=r2[:], in0=O2[:], in1=r2[:], op=ALU.mult), reads=[O2b, b_e], writes=[b_e])
            fw.op("dve", lambda: nc.vector.scalar_tensor_tensor(out=o_sb[:], in0=r2[:], scalar=nlam[:, 0:1], in1=r1[:],
                                                                op0=ALU.mult, op1=ALU.add), reads=[b_e, b_l], writes=[b_e])
            fw.op("act", lambda: nc.scalar.activation(out=osq[:], in_=o_sb[:], func=AF.Square), reads=[b_e], writes=[b_e])
            st, stb = k.bank("g")
            fw.op("pe", lambda st=st: nc.tensor.matmul(st[:], lhsT=ones_e[:], rhs=osq[:], start=True, stop=True),
                  reads=[b_e, b_c], writes=[stb])
            fw.op("act", lambda st=st: nc.scalar.activation(out=rs[:], in_=st[:], func=AF.Sqrt, bias=epsb[:, 0:1]),
                  reads=[stb, b_c, b_e], writes=[b_e])
            fw.op("dve", lambda: nc.vector.reciprocal(out=rs[:], in_=rs[:]), reads=[b_e], writes=[b_e])
            fw.op("dve", lambda: nc.vector.tensor_tensor(out=o_sb[:], in0=o_sb[:], in1=rs[:], op=ALU.mult), reads=[b_e], writes=[b_e])
            jj = k.rot("ystg", 2)
            fw.op("act", lambda jj=jj: nc.scalar.activation(out=yst[jj][:], in_=o_sb[:], func=AF.Copy, scale=gsub[:, 0:1]),
                  reads=[b_e, b_l], writes=[ystb[jj]])
            fw.dma("sp", yT[1, :, q0:q0 + TT], yst[jj][:], reads=[ystb[jj]], writes=[outb])
        fw.barrier()


def build_L2():
    k = K("L2")
    qT = k.inp("qT", [128, SEQ], BF16)
    kT = k.inp("kT", [128, SEQ], BF16)
    v_tok = k.inp("v_tok", [SEQ, 128], BF16)
    u_tok = k.inp("u_tok", [SEQ, 128], BF16)
    tri = k.inp("tri", [128, 128], BF16)
    bands = k.inp("bands", [128, 3, 128], BF16)
    wpool = k.inp("wpool", [128, 128])
    pscale = k.inp("pscale", [128, 1])
    lam = k.inp("lam", [128, 256])
    subg = k.inp("subg", [128, 1])
    yT = k.out("yT", [2, 128, SEQ], BF16)
    with k.es as es:
        emit_mixer_even(k, es, qT, kT, v_tok, u_tok, tri, bands, wpool, pscale, lam, subg, yT)
        k.fw.finish()
    return k


POOL_WINDOWS = (2, 4, 8, 16)


def band_mats(w):
    s = np.arange(128)[:, None]
    t = np.arange(128)[None, :]
    d = t - s
    bd = np.where((d >= 0) & (d < w), 1.0 / w, 0.0) - (d == 0)
    bo = np.where((t + 128 - s) < w, 1.0 / w, 0.0)
    cnt = np.minimum(t + 1, w)
    b0 = np.where((d >= 0) & (d < w), 1.0 / cnt, 0.0) - (d == 0)
    return np.stack([bd, bo, b0], axis=1).astype(np.float32).astype(NPBF)


TRI = (np.arange(128)[:, None] <= np.arange(128)[None, :]).astype(np.float32).astype(NPBF)


def emit_outproj(k, xs, mod, w_out, ysrc, gate_src=None):
    nc, fw = k.nc, k.fw
    with ExitStack() as es:
        wt = k.sb(es, "wout", [128, KC, D], BF16)
        wb = Buf()
        wv = w_out.rearrange("(kc p) n -> p kc n", p=128)
        for g in range(2):
            fw.dma("pool", wt[:, g * 4:(g + 1) * 4, :], wv[:, g * 4:(g + 1) * 4, :], writes=[wb])
        yt = [k.sb(es, "yt", [128, KC, TT], BF16) for _ in range(2)]
        ytb = [Buf(), Buf()]
        gt = [k.sb(es, "ygt", [128, KC, TT], BF16) for _ in range(2)] if gate_src is not None else None
        yv = ysrc.rearrange("c p t -> p c t")
        gv = gate_src.rearrange("c p t -> p c t") if gate_src is not None else None
        for t in range(NTILE):
            j = k.rot("yt", 2)
            tok = slice(t * TT, (t + 1) * TT)
            fw.dma("sp", yt[j][:], yv[:, :, tok], writes=[ytb[j]])
            if gv is not None:
                fw.dma("sp", gt[j][:], gv[:, :, tok], writes=[ytb[j]])
                fw.op("dve", lambda j=j: nc.vector.tensor_tensor(out=yt[j][:], in0=yt[j][:], in1=gt[j][:], op=ALU.mult),
                      reads=[ytb[j]], writes=[ytb[j]])
            for dc in range(KC):
                pd, pdb = k.bank("d")
                for kc in range(KC):
                    fw.op("pe", lambda kc=kc, dc=dc, j=j, pd=pd: nc.tensor.matmul(
                        pd[:], lhsT=wt[:, kc, dc * 128:(dc + 1) * 128], rhs=yt[j][:, kc, :],
                        start=(kc == 0), stop=(kc == KC - 1)), reads=[wb, ytb[j]], writes=[pdb])
                xsl = xs.t[:, dc, tok]
                fw.op("dve", lambda pd=pd, dc=dc, xsl=xsl: nc.vector.scalar_tensor_tensor(
                    out=xsl, in0=pd[:], scalar=mod["gt"][1][:, dc:dc + 1], in1=xsl, op0=ALU.mult, op1=ALU.add),
                    reads=[pdb, mod["buf"], xs.b[t][dc]], writes=[xs.b[t][dc]])
        fw.barrier()


def emit_inproj_odd(k, cst, ns, xs, mod, w_in, pf, pt, so, pg):
    nc, fw = k.nc, k.fw
    with ExitStack() as es:
        hT = k.sb(es, "hTo", [128, KC, 2 * TT], BF16)
        hb = [Buf(), Buf()]
        wt = k.sb(es, "wino", [128, KC, 3080], BF16)
        wb = [Buf() for _ in range(7)]
        wv = w_in.rearrange("(kc p) n -> p kc n", p=128)
        for g in range(6):
            fw.dma("pool", wt[:, :, g * 512:(g + 1) * 512], wv[:, :, g * 512:(g + 1) * 512], writes=[wb[g]])
        with nc.allow_non_contiguous_dma(reason="8 gate columns"):
            fw.dma("pool", wt[:, :, 3072:3080], wv[:, :, 3072:3080], writes=[wb[6]])
        stg_t = [k.sb(es, "stg_to", [128, 1536], BF16) for _ in range(2)]
        stg_tb = [Buf(), Buf()]
        stg_f = [k.sb(es, "stg_fo", [128, 2 * TT], BF16) for _ in range(2)]
        stg_fb = [Buf(), Buf()]
        stg_g = k.sb(es, "stg_g", [128, 2 * TT], F32)
        stg_gb = Buf()
        outb = Buf()
        for st in range(2):
            for tl in range(2):
                emit_norm_tile(k, cst, ns, xs, st * 2 + tl, hT, tl * TT, hb[tl], mod["gs"][1], mod["sh"][1], mod["buf"])
            for tc in range(8):
                tl = tc // 4
                j = k.rot("stg_to", 2)
                for part, g in enumerate((1, 2, 3)):
                    ps, psb = k.bank("g" if part % 2 == 0 else "u")
                    for kc in range(KC):
                        fw.op("pe", lambda kc=kc, tc=tc, g=g, ps=ps: nc.tensor.matmul(
                            ps[:], lhsT=hT[:, kc, tc * 128:(tc + 1) * 128], rhs=wt[:, kc, g * 512:(g + 1) * 512],
                            start=(kc == 0), stop=(kc == KC - 1)), reads=[hb[tl], wb[g]], writes=[psb])
                    if part == 1:
                        fw.op("act", lambda j=j, part=part, ps=ps: nc.scalar.copy(out=stg_t[j][:, part * 512:(part + 1) * 512], in_=ps[:]),
                              reads=[psb], writes=[stg_tb[j]])
                    else:
                        fw.op("dve", lambda j=j, part=part, ps=ps: nc.vector.tensor_copy(out=stg_t[j][:, part * 512:(part + 1) * 512], in_=ps[:]),
                              reads=[psb], writes=[stg_tb[j]])
                r0 = st * 2 * TT + tc * 128
                fw.dma("sp", pt[r0:r0 + 128, :], stg_t[j][:], reads=[stg_tb[j]], writes=[outb])
            for ch in list(range(8)) + list(range(16, 24)) + [24]:
                c0 = ch * 128 if ch < 24 else 2952
                g = min(c0 // 512, 5)
                rd = [wb[g]] + ([wb[6]] if ch == 24 else []) + ([wb[5]] if ch == 24 else [])
                j = k.rot("stg_fo", 2)
                for tl in range(2):
                    ps, psb = k.bank("d")
                    for kc in range(KC):
                        fw.op("pe", lambda kc=kc, c0=c0, tl=tl, ps=ps: nc.tensor.matmul(
                            ps[:], lhsT=wt[:, kc, c0:c0 + 128], rhs=hT[:, kc, tl * TT:(tl + 1) * TT],
                            start=(kc == 0), stop=(kc == KC - 1)), reads=[hb[tl]] + rd, writes=[psb])
                    if ch == 24:
                        fw.op("dve", lambda tl=tl, ps=ps: nc.vector.tensor_copy(out=stg_g[:, tl * TT:(tl + 1) * TT], in_=ps[:]),
                              reads=[psb], writes=[stg_gb])
                    elif ch >= 16:
                        fw.op("act", lambda j=j, tl=tl, ps=ps: nc.scalar.activation(out=stg_f[j][:, tl * TT:(tl + 1) * TT], in_=ps[:], func=AF.Sigmoid),
                              reads=[psb], writes=[stg_fb[j]])
                    elif tl == 0:
                        fw.op("act", lambda j=j, tl=tl, ps=ps: nc.scalar.copy(out=stg_f[j][:, tl * TT:(tl + 1) * TT], in_=ps[:]),
                              reads=[psb], writes=[stg_fb[j]])
                    else:
                        fw.op("dve", lambda j=j, tl=tl, ps=ps: nc.vector.tensor_copy(out=stg_f[j][:, tl * TT:(tl + 1) * TT], in_=ps[:]),
                              reads=[psb], writes=[stg_fb[j]])
                tsl = slice(st * 2 * TT, (st + 1) * 2 * TT)
                if ch == 24:
                    fw.dma("sp", pg[:, tsl], stg_g[120:128, :], reads=[stg_gb], writes=[outb])
                elif ch >= 16:
                    fw.dma("sp", so[ch - 16, :, tsl], stg_f[j][:], reads=[stg_fb[j]], writes=[outb])
                else:
                    fw.dma("sp", pf[ch, :, tsl], stg_f[j][:], reads=[stg_fb[j]], writes=[outb])
        fw.barrier()


def emit_final_norm(k, cst, ns, xs, fg_in, outT):
    nc, fw = k.nc, k.fw
    with ExitStack() as es:
        fg = k.sb(es, "fg", [128, 8], F32)
        z = k.sb(es, "zz", [128, 8], F32)
        fb = Buf()
        fw.dma("sp", fg[:], fg_in, writes=[fb])
        fw.op("dve", lambda: nc.vector.memset(z[:], 0.0), writes=[fb])
        ho = [k.sb(es, "hfin", [128, KC, TT], F32) for _ in range(2)]
        hob = [Buf(), Buf()]
        ov = outT.rearrange("(c p) t -> p c t", p=128)
        for t in range(NTILE):
            j = t % 2
            emit_norm_tile(k, cst, ns, xs, t, ho[j], 0, hob[j], fg, z, fb)
            fw.dma("sp", ov[:, :, t * TT:(t + 1) * TT], ho[j][:], reads=[hob[j]])
        fw.barrier()


def build_L3():
    k = K("L3")
    xin = k.inp("xT", [D, NT])
    c_in = k.inp("c", [128, 8])
    wmod0 = k.inp("w_mod0", [D, 9 * D]); bmod0 = k.inp("b_mod0", [128, 72]); ng0 = k.inp("ng0", [128, 24])
    wmod1 = k.inp("w_mod1", [D, 9 * D]); bmod1 = k.inp("b_mod1", [128, 72]); ng1 = k.inp("ng1", [128, 24])
    w_out = k.inp("w_out", [D, D])
    ycat = k.inp("ycat", [8, 128, NT], BF16)
    wg0 = k.inp("wg0", [D, DFF]); wu0 = k.inp("wu0", [D, DFF]); wd0 = k.inp("wd0", [DFF, D])
    wg1 = k.inp("wg1", [D, DFF]); wu1 = k.inp("wu1", [D, DFF]); wd1 = k.inp("wd1", [DFF, D])
    w_in = k.inp("w_in", [D, 3080])
    xout = k.out("xo", [D, NT])
    pf = k.out("pf", [8, 128, NT], BF16)
    pt = k.out("pt", [NT, 1536], BF16)
    so = k.out("so", [8, 128, NT], BF16)
    pg = k.out("pg", [8, NT])
    with k.es as es:
        cst = load_consts(k, es)
        xs = XS(k, es)
        xs.load(k, xin)
        ns = NormScratch(k, es)
        mod0 = emit_mod(k, es, c_in, wmod0, bmod0, ng0, cst)
        emit_outproj(k, xs, mod0, w_out, ycat)
        emit_ffn(k, es, cst, ns, xs, mod0, 2, wg0, wu0, wd0)
        mod1 = emit_mod(k, es, c_in, wmod1, bmod1, ng1, cst)
        emit_ffn(k, es, cst, ns, xs, mod1, 0, wg1, wu1, wd1)
        emit_inproj_odd(k, cst, ns, xs, mod1, w_in, pf, pt, so, pg)
        xs.store(k, xout)
        k.fw.finish()
    return k


def build_L5():
    k = K("L5")
    xin = k.inp("xT", [D, NT])
    c_in = k.inp("c", [128, 8])
    wmod1 = k.inp("w_mod1", [D, 9 * D]); bmod1 = k.inp("b_mod1", [128, 72]); ng1 = k.inp("ng1", [128, 24])
    w_out = k.inp("w_out", [D, D])
    hcat = k.inp("hcat", [8, 128, NT], BF16)
    so = k.inp("so", [8, 128, NT], BF16)
    wg1 = k.inp("wg1", [D, DFF]); wu1 = k.inp("wu1", [D, DFF]); wd1 = k.inp("wd1", [DFF, D])
    fg = k.inp("fg", [128, 8])
    outT = k.out("outT", [D, NT])
    with k.es as es:
        cst = load_consts(k, es)
        xs = XS(k, es)
        xs.load(k, xin)
        ns = NormScratch(k, es)
        mod1 = emit_mod(k, es, c_in, wmod1, bmod1, ng1, cst)
        emit_outproj(k, xs, mod1, w_out, hcat, gate_src=so)
        emit_ffn(k, es, cst, ns, xs, mod1, 2, wg1, wu1, wd1)
        emit_final_norm(k, cst, ns, xs, fg, outT)
        k.fw.finish()
    return k


def emit_mlstm(k, es0, qT, kT, k_tok, v_tok, ig_in, fg_in, bias_in, ng_in, tri_in, ident_in, htT):
    nc, fw = k.nc, k.fw
    NCH = SEQ // 128
    SC = 128.0 ** -0.5
    with ExitStack() as es:
        qt = k.sb(es, "m_qt", [128, SEQ], BF16)
        kt = k.sb(es, "m_kt", [128, SEQ], BF16)
        ktk = k.sb(es, "m_ktok", [128, NCH, 128], BF16)
        vtk = k.sb(es, "m_vtok", [128, NCH, 256], BF16)
        b_q = [Buf() for _ in range(16)]
        b_k = [Buf() for _ in range(4)]
        b_kt = [Buf() for _ in range(4)]
        b_v = [Buf() for _ in range(4)]
        b_c = Buf()
        tri = k.sb(es, "m_tri", [128, 128], F32)
        tri_bf = k.sb(es, "m_tri_bf", [128, 128], BF16)
        ident = k.sb(es, "m_ident", [128, 128], F32)
        ones_f = k.sb(es, "m_ones_f", [128, 128], F32)
        ones_b = k.sb(es, "m_ones_b", [128, 128], BF16)
        ones_v = k.sb(es, "m_ones_v", [128, 128], BF16)
        epsb = k.sb(es, "m_eps", [128, 1], F32)
        ig = k.sb(es, "m_ig", [128, NCH], F32)
        fg = k.sb(es, "m_fg", [128, NCH], F32)
        bias = k.sb(es, "m_bias", [128, 2], F32)
        ng = k.sb(es, "m_ng", [128, 2], F32)
        fw.dma("sp", tri[:], tri_in, writes=[b_c])
        fw.dma("sp", ident[:], ident_in, writes=[b_c])
        fw.dma("sp", ig[:], ig_in, writes=[b_c])
        fw.dma("sp", fg[:], fg_in, writes=[b_c])
        fw.dma("sp", bias[:], bias_in, writes=[b_c])
        fw.dma("sp", ng[:], ng_in, writes=[b_c])
        fw.op("dve", lambda: nc.vector.memset(ones_f[:], 1.0), writes=[b_c])
        fw.op("dve", lambda: nc.vector.memset(ones_b[:], 1.0), writes=[b_c])
        fw.op("dve", lambda: nc.vector.memset(ones_v[:], 1.0 / 256.0), writes=[b_c])
        fw.op("dve", lambda: nc.vector.memset(epsb[:], EPS), writes=[b_c])
        fw.op("dve", lambda: nc.vector.tensor_copy(out=tri_bf[:], in_=tri[:]), reads=[b_c], writes=[b_c])
        ktv = k_tok.rearrange("(c p) e -> p c e", p=128)
        vtv = v_tok.rearrange("(c p) e -> p c e", p=128)
        for i in range(4):
            sl = slice(i * 2048, (i + 1) * 2048)
            fw.dma("sp", qt[:, sl], qT[:, sl], writes=b_q[i * 4:(i + 1) * 4])
            fw.dma("sp", kt[:, sl], kT[:, sl], writes=[b_k[i]])
            fw.dma("sp", ktk[:, i * 16:(i + 1) * 16, :], ktv[:, i * 16:(i + 1) * 16, :], writes=[b_kt[i]])
            fw.dma("sp", vtk[:, i * 16:(i + 1) * 16, :], vtv[:, i * 16:(i + 1) * 16, :], writes=[b_v[i]])
        logf = k.sb(es, "m_logf", [128, NCH], F32)
        bcol = k.sb(es, "m_bcol", [128, NCH], F32)
        rcol = k.sb(es, "m_rcol", [128, NCH], F32)
        wcol = k.sb(es, "m_wcol", [128, NCH], F32)
        ebl = k.sb(es, "m_ebl", [128, NCH], F32)
        b_g = Buf()
        fw.op("dve", lambda: nc.vector.tensor_scalar(out=ig[:], in0=ig[:], scalar1=bias[:, 0:1], scalar2=None, op0=ALU.add),
              reads=[b_c], writes=[b_g])
        fw.op("dve", lambda: nc.vector.tensor_scalar(out=fg[:], in0=fg[:], scalar1=bias[:, 1:2], scalar2=None, op0=ALU.add),
              reads=[b_c, b_g], writes=[b_g])
        fw.op("act", lambda: nc.scalar.activation(out=logf[:], in_=fg[:], func=AF.Exp, scale=-1.0), reads=[b_g], writes=[b_g])
        fw.op("act", lambda: nc.scalar.activation(out=logf[:], in_=logf[:], func=AF.Ln, bias=1.0), reads=[b_g], writes=[b_g])
        fw.op("dve", lambda: nc.vector.tensor_scalar(out=logf[:], in0=logf[:], scalar1=-1.0, scalar2=None, op0=ALU.mult),
              reads=[b_g], writes=[b_g])
        pa, pab = k.ps[0], k.psb[0]
        fw.op("pe", lambda: nc.tensor.matmul(pa[:, 0:NCH], lhsT=tri[:], rhs=logf[:], start=True, stop=True),
              reads=[b_g, b_c], writes=[pab])
        fw.op("pe", lambda: nc.tensor.matmul(pa[:, NCH:2 * NCH], lhsT=ones_f[:], rhs=logf[:], start=True, stop=True),
              reads=[b_g, b_c], writes=[pab])
        fw.op("dve", lambda: nc.vector.tensor_copy(out=bcol[:], in_=pa[:, 0:NCH]), reads=[pab], writes=[b_g])
        fw.op("dve", lambda: nc.vector.tensor_tensor(out=rcol[:], in0=ig[:], in1=bcol[:], op=ALU.subtract), reads=[b_g], writes=[b_g])
        fw.op("dve", lambda: nc.vector.tensor_tensor(out=wcol[:], in0=rcol[:], in1=pa[:, NCH:2 * NCH], op=ALU.add), reads=[b_g, pab], writes=[b_g])
        fw.op("act", lambda: nc.scalar.activation(out=rcol[:], in_=rcol[:], func=AF.Exp), reads=[b_g], writes=[b_g])
        fw.op("act", lambda: nc.scalar.activation(out=wcol[:], in_=wcol[:], func=AF.Exp), reads=[b_g], writes=[b_g])
        fw.op("act", lambda: nc.scalar.activation(out=ebl[:], in_=pa[:, NCH:2 * NCH], func=AF.Exp), reads=[b_g, pab], writes=[b_g])
        fw.op("dve", lambda: nc.vector.tensor_scalar(out=rcol[:], in0=rcol[:], scalar1=SC, scalar2=None, op0=ALU.mult), reads=[b_g], writes=[b_g])
        fw.op("dve", lambda: nc.vector.tensor_scalar(out=wcol[:], in0=wcol[:], scalar1=SC, scalar2=None, op0=ALU.mult), reads=[b_g], writes=[b_g])
        dg = [k.sb(es, "m_dg", [128, 128], F32) for _ in range(2)]
        dgb = [Buf(), Buf()]
        ebt = [k.sb(es, "m_ebt", [128, TT], F32) for _ in range(2)]
        ebtb = [Buf(), Buf()]
        for g4 in range(NCH // 4):
            ps, psb = k.bank("u")
            for cc in range(4):
                c = g4 * 4 + cc
                j = k.rot("m_dg", 2)
                fw.op("dve", lambda c=c, j=j: nc.vector.tensor_scalar(out=dg[j][:], in0=ident[:], scalar1=bcol[:, c:c + 1], scalar2=None, op0=ALU.mult),
                      reads=[b_g, b_c], writes=[dgb[j]])
                fw.op("pe", lambda cc=cc, j=j, ps=ps: nc.tensor.matmul(ps[:, cc * 128:(cc + 1) * 128], lhsT=ones_f[:], rhs=dg[j][:], start=True, stop=True),
                      reads=[dgb[j], b_c], writes=[psb])
            j2 = k.rot("m_ebt", 2)
            fw.op("act", lambda j2=j2, ps=ps: nc.scalar.activation(out=ebt[j2][:], in_=ps[:], func=AF.Exp), reads=[psb], writes=[ebtb[j2]])
            sl = slice(g4 * TT, (g4 + 1) * TT)
            fw.op("dve", lambda j2=j2, sl=sl: nc.vector.tensor_tensor(out=qt[:, sl], in0=qt[:, sl], in1=ebt[j2][:], op=ALU.mult),
                  reads=[ebtb[j2], b_q[g4]], writes=[b_q[g4]])
        st_f = k.sb(es, "m_stf", [128, 384], F32)
        st_b = [k.sb(es, "m_stb", [128, 384], BF16) for _ in range(2)]
        stfb = Buf()
        stbb = [Buf(), Buf()]
        ssb = [k.sb(es, "m_ssb", [128, 128], BF16) for _ in range(2)]
        ssbb = [Buf(), Buf()]
        kw = [k.sb(es, "m_kw", [128, 128], BF16) for _ in range(2)]
        kwb = [Buf(), Buf()]
        rden = [k.sb(es, "m_rden", [128, 128], F32) for _ in range(2)]
        rdb = [Buf(), Buf()]
        hsb = [k.sb(es, "m_hsb", [128, 2, TT], F32) for _ in range(2)]
        hsbb = [Buf(), Buf()]
        hsq = k.sb(es, "m_hsq", [128, 2, TT], BF16)
        hrs = k.sb(es, "m_hrs", [128, TT], F32)
        hout = [k.sb(es, "m_hout", [128, 2, TT], BF16) for _ in range(2)]
        houtb = [Buf(), Buf()]
        b_n = Buf()
        outb = Buf()
        fw.op("dve", lambda: nc.vector.memset(st_f[:], 0.0), writes=[stfb])
        for c in range(NCH):
            csl = slice(c * 128, (c + 1) * 128)
            g4, cc = c // 4, c % 4
            hj = g4 % 2
            pS, pSb = k.bank("g")
            fw.op("pe", lambda pS=pS, csl=csl: nc.tensor.matmul(pS[:, 0:128], lhsT=kt[:, csl], rhs=qt[:, csl], start=True, stop=True),
                  reads=[b_k[c // 16], b_q[g4]], writes=[pSb])
            j = k.rot("m_ssb", 2)
            fw.op("dve", lambda pS=pS, j=j, c=c: nc.vector.scalar_tensor_tensor(
                out=ssb[j][:], in0=pS[:, 0:128], scalar=rcol[:, c:c + 1], in1=tri_bf[:], op0=ALU.mult, op1=ALU.mult),
                reads=[pSb, b_g, b_c], writes=[ssbb[j]])
            pN, pNb = k.bank("d")
            sbuf_state = st_b[c % 2]
            for vh in range(2):
                fw.op("pe", lambda pN=pN, vh=vh, j=j, c=c: nc.tensor.matmul(
                    pN[:, vh * 128:(vh + 1) * 128], lhsT=vtk[:, c, vh * 128:(vh + 1) * 128], rhs=ssb[j][:], start=True, stop=(c == 0)),
                    reads=[b_v[c // 16], ssbb[j]], writes=[pNb])
                if c > 0:
                    fw.op("pe", lambda pN=pN, vh=vh, csl=csl, sbuf_state=sbuf_state: nc.tensor.matmul(
                        pN[:, vh * 128:(vh + 1) * 128], lhsT=sbuf_state[:, vh * 128:(vh + 1) * 128], rhs=qt[:, csl], start=False, stop=True),
                        reads=[stbb[c % 2], b_q[g4]], writes=[pNb])
            fw.op("pe", lambda pN=pN, j=j: nc.tensor.matmul(pN[:, 256:384], lhsT=ones_b[:], rhs=ssb[j][:], start=True, stop=(c == 0)),
                  reads=[b_c, ssbb[j]], writes=[pNb])
            if c > 0:
                fw.op("pe", lambda pN=pN, csl=csl, sbuf_state=sbuf_state: nc.tensor.matmul(
                    pN[:, 256:384], lhsT=sbuf_state[:, 256:384], rhs=qt[:, csl], start=False, stop=True),
                    reads=[stbb[c % 2], b_q[g4]], writes=[pNb])
            jr = k.rot("m_rden", 2)
            fw.op("dve", lambda pN=pN, jr=jr: nc.vector.tensor_scalar(out=rden[jr][:], in0=pN[:, 256:384], scalar1=-1.0, scalar2=1.0,
                                                                      op0=ALU.mult, op1=ALU.max), reads=[pNb], writes=[rdb[jr]])
            fw.op("dve", lambda pN=pN, jr=jr: nc.vector.tensor_tensor(out=rden[jr][:], in0=pN[:, 256:384], in1=rden[jr][:], op=ALU.max),
                  reads=[pNb, rdb[jr]], writes=[rdb[jr]])
            fw.op("dve", lambda jr=jr: nc.vector.reciprocal(out=rden[jr][:], in_=rden[jr][:]), reads=[rdb[jr]], writes=[rdb[jr]])
            for vh in range(2):
                fw.op("dve", lambda pN=pN, vh=vh, jr=jr, hj=hj, cc=cc: nc.vector.tensor_tensor(
                    out=hsb[hj][:, vh, cc * 128:(cc + 1) * 128], in0=pN[:, vh * 128:(vh + 1) * 128], in1=rden[jr][:], op=ALU.mult),
                    reads=[pNb, rdb[jr]], writes=[hsbb[hj]])
            if c < NCH - 1:
                jk = k.rot("m_kw", 2)
                fw.op("act", lambda jk=jk, c=c: nc.scalar.activation(out=kw[jk][:], in_=ktk[:, c, :], func=AF.Copy, scale=wcol[:, c:c + 1]),
                      reads=[b_kt[c // 16], b_g], writes=[kwb[jk]])
                pC, pCb = k.bank("u")
                fw.op("pe", lambda pC=pC, jk=jk, c=c: nc.tensor.matmul(pC[:, 0:256], lhsT=kw[jk][:], rhs=vtk[:, c, :], start=True, stop=True),
                      reads=[kwb[jk], b_v[c // 16]], writes=[pCb])
                fw.op("pe", lambda pC=pC, jk=jk: nc.tensor.matmul(pC[:, 256:384], lhsT=kw[jk][:], rhs=ones_b[:], start=True, stop=True),
                      reads=[kwb[jk], b_c], writes=[pCb])
                fw.op("dve", lambda pC=pC, c=c: nc.vector.scalar_tensor_tensor(
                    out=st_f[:], in0=st_f[:], scalar=ebl[:, c:c + 1], in1=pC[:, 0:384], op0=ALU.mult, op1=ALU.add),
                    reads=[pCb, b_g, stfb], writes=[stfb])
                nb = (c + 1) % 2
                fw.op("act", lambda nb=nb: nc.scalar.copy(out=st_b[nb][:], in_=st_f[:]), reads=[stfb], writes=[stbb[nb]])
            if cc == 3:
                fw.op("act", lambda hj=hj: nc.scalar.activation(out=hsq[:], in_=hsb[hj][:], func=AF.Square), reads=[hsbb[hj], b_n], writes=[b_n])
                pR, pRb = k.bank("s")
                for vh in range(2):
                    fw.op("pe", lambda pR=pR, vh=vh: nc.tensor.matmul(pR[:], lhsT=ones_v[:], rhs=hsq[:, vh, :], start=(vh == 0), stop=(vh == 1)),
                          reads=[b_n, b_c], writes=[pRb])
                fw.op("act", lambda pR=pR: nc.scalar.activation(out=hrs[:], in_=pR[:], func=AF.Sqrt, bias=epsb[:, 0:1]),
                      reads=[pRb, b_c, b_n], writes=[b_n])
                fw.op("dve", lambda: nc.vector.reciprocal(out=hrs[:], in_=hrs[:]), reads=[b_n], writes=[b_n])
                oj = k.rot("m_hout", 2)
                for vh in range(2):
                    fw.op("dve", lambda hj=hj, vh=vh, oj=oj: nc.vector.scalar_tensor_tensor(
                        out=hout[oj][:, vh, :], in0=hsb[hj][:, vh, :], scalar=ng[:, vh:vh + 1], in1=hrs[:], op0=ALU.mult, op1=ALU.mult),
                        reads=[hsbb[hj], b_n, b_c], writes=[houtb[oj]])
                fw.dma("sp", htT.rearrange("v p t -> p v t")[:, :, g4 * TT:(g4 + 1) * TT], hout[oj][:], reads=[houtb[oj]], writes=[outb])
        fw.barrier()


def build_L4():
    k = K("L4")
    qT = k.inp("qT", [128, SEQ], BF16)
    kT = k.inp("kT", [128, SEQ], BF16)
    k_tok = k.inp("k_tok", [SEQ, 128], BF16)
    v_tok = k.inp("v_tok", [SEQ, 256], BF16)
    ig = k.inp("ig", [128, SEQ // 128])
    fg = k.inp("fg", [128, SEQ // 128])
    bias = k.inp("bias", [128, 2])
    ng = k.inp("ng", [128, 2])
    tri = k.inp("tri", [128, 128])
    ident = k.inp("ident", [128, 128])
    htT = k.out("htT", [2, 128, SEQ], BF16)
    with k.es as es:
        emit_mlstm(k, es, qT, kT, k_tok, v_tok, ig, fg, bias, ng, tri, ident, htT)
        k.fw.finish()
    return k


def _tok(r):
    b, j = r // 4, r % 4
    return b, slice(j * NT, (j + 1) * NT)


def kernel_unfused(x, c, w_mod, b_mod, norm_g, w_ffn_gate, w_ffn_up, w_ffn_down, w_in_even, w_pool, pool_scale,
                   diff_lambda, diff_subln_g, w_out_even, w_in_odd, b_gates_odd, mlstm_norm_g, w_out_odd, final_g):
    f32 = np.float32
    A = lambda a: np.ascontiguousarray(np.asarray(a, dtype=f32))
    x = A(x); c = A(c); w_mod = A(w_mod); b_mod = A(b_mod); norm_g = A(norm_g)
    w_ffn_gate = A(w_ffn_gate); w_ffn_up = A(w_ffn_up); w_ffn_down = A(w_ffn_down)
    w_in_even = A(w_in_even); w_pool = A(w_pool); pool_scale = A(pool_scale); diff_lambda = A(diff_lambda)
    diff_subln_g = A(diff_subln_g); w_out_even = A(w_out_even); w_in_odd = A(w_in_odd); b_gates_odd = A(b_gates_odd)
    mlstm_norm_g = A(mlstm_norm_g); w_out_odd = A(w_out_odd); final_g = A(final_g)
    R = range(NCORES)
    k1 = get_prog("L1", build_L1)
    maps = []
    for r in R:
        b, sl = _tok(r)
        maps.append({"xT": np.ascontiguousarray(x[b, sl, :].T), "c": fm(c[b]), "w_mod0": w_mod[0], "b_mod0": fm(b_mod[0]),
                     "ng0": fm(norm_g[0]), "wg": w_ffn_gate[0, 0], "wu": w_ffn_up[0, 0], "wd": w_ffn_down[0, 0],
                     "w_in": w_in_even[0]})
    r1 = run(k1, maps)
    k2 = get_prog("L2", build_L2)
    maps = []
    for r in R:
        b, h = r // 4, r % 4
        src = [r1[b * 4 + j] for j in range(4)]
        maps.append({
            "qT": np.ascontiguousarray(np.concatenate([s_["pf"][h] for s_ in src], axis=1)),
            "kT": np.ascontiguousarray(np.concatenate([s_["pf"][4 + h] for s_ in src], axis=1)),
            "u_tok": np.ascontiguousarray(np.concatenate([s_["pt"][:, h * 128:(h + 1) * 128] for s_ in src], axis=0)),
            "v_tok": np.ascontiguousarray(np.concatenate([s_["pt"][:, 512 + h * 128:512 + (h + 1) * 128] for s_ in src], axis=0)),
            "tri": TRI, "bands": band_mats(POOL_WINDOWS[h]),
            "wpool": w_pool[0, h], "pscale": np.ascontiguousarray(pool_scale[0, h * 128:(h + 1) * 128, None]),
            "lam": np.ascontiguousarray(np.broadcast_to(diff_lambda[0].reshape(1, 256), (128, 256))),
            "subg": np.ascontiguousarray(diff_subln_g[0][:, None]),
        })
    r2 = run(k2, maps)
    k3 = get_prog("L3", build_L3)
    maps = []
    for r in R:
        b, sl = _tok(r)
        ycat = np.stack([r2[b * 4 + g]["yT"][0][:, sl] for g in range(4)] + [r2[b * 4 + g]["yT"][1][:, sl] for g in range(4)], axis=0)
        maps.append({"xT": r1[r]["xo"], "c": fm(c[b]),
                     "w_mod0": w_mod[0], "b_mod0": fm(b_mod[0]), "ng0": fm(norm_g[0]),
                     "w_mod1": w_mod[1], "b_mod1": fm(b_mod[1]), "ng1": fm(norm_g[1]),
                     "w_out": w_out_even[0], "ycat": np.ascontiguousarray(ycat),
                     "wg0": w_ffn_gate[0, 1], "wu0": w_ffn_up[0, 1], "wd0": w_ffn_down[0, 1],
                     "wg1": w_ffn_gate[1, 0], "wu1": w_ffn_up[1, 0], "wd1": w_ffn_down[1, 0],
                     "w_in": w_in_odd[0]})
    r3 = run(k3, maps)
    k4 = get_prog("L4", build_L4)
    maps = []
    for r in R:
        b, h = r // 4, r % 4
        src = [r3[b * 4 + j] for j in range(4)]
        ig = np.concatenate([s_["pg"][h] for s_ in src], axis=0)
        fg = np.concatenate([s_["pg"][4 + h] for s_ in src], axis=0)
        maps.append({
            "qT": np.ascontiguousarray(np.concatenate([s_["pf"][h] for s_ in src], axis=1)),
            "kT": np.ascontiguousarray(np.concatenate([s_["pf"][4 + h] for s_ in src], axis=1)),
            "k_tok": np.ascontiguousarray(np.concatenate([s_["pt"][:, h * 128:(h + 1) * 128] for s_ in src], axis=0)),
            "v_tok": np.ascontiguousarray(np.concatenate([s_["pt"][:, 512 + h * 256:512 + (h + 1) * 256] for s_ in src], axis=0)),
            "ig": np.ascontiguousarray(ig.reshape(64, 128).T), "fg": np.ascontiguousarray(fg.reshape(64, 128).T),
            "bias": np.ascontiguousarray(np.broadcast_to(b_gates_odd[0][:, h][None, :], (128, 2))),
            "ng": np.ascontiguousarray(mlstm_norm_g[0][h * 256:(h + 1) * 256].reshape(2, 128).T),
            "tri": TRI.astype(f32), "ident": np.eye(128, dtype=f32),
        })
    r4 = run(k4, maps)
    k5 = get_prog("L5", build_L5)
    maps = []
    for r in R:
        b, sl = _tok(r)
        hcat = np.stack([r4[b * 4 + h]["htT"][vh][:, sl] for h in range(4) for vh in range(2)], axis=0)
        maps.append({"xT": r3[r]["xo"], "c": fm(c[b]), "w_mod1": w_mod[1], "b_mod1": fm(b_mod[1]), "ng1": fm(norm_g[1]),
                     "w_out": w_out_odd[0], "hcat": np.ascontiguousarray(hcat), "so": r3[r]["so"],
                     "wg1": w_ffn_gate[1, 1], "wu1": w_ffn_up[1, 1], "wd1": w_ffn_down[1, 1], "fg": fm(final_g)})
    r5 = run(k5, maps)
    out = np.empty((2, SEQ, D), dtype=f32)
    for r in R:
        b, sl = _tok(r)
        out[b, sl, :] = r5[r]["outT"].T
    return out
```
